# Optimizing a Trainium2 kernel written in Bass

```python
import jax, jax.numpy as jnp
from jax import lax
import numpy as np

D_MODEL = 1024
BATCH = 2
SEQ = 8192
DEPTH = 4

N_MIXERS = 2
N_MLSTM_LAYERS = (DEPTH + N_MIXERS - 1) // N_MIXERS
N_ATTN_LAYERS = DEPTH // N_MIXERS

MLSTM_HEADS = 4
MLSTM_QK_DIM = D_MODEL // (2 * MLSTM_HEADS)
MLSTM_V_DIM = D_MODEL // MLSTM_HEADS
MLSTM_CHUNK = 128
MLSTM_N_GATES = 4 * MLSTM_HEADS
MLSTM_IN_WIDTH = 2 * MLSTM_HEADS * MLSTM_QK_DIM + 2 * D_MODEL + MLSTM_N_GATES
FGATE_BIAS_LO = 3.0
FGATE_BIAS_HI = 6.0

ATTN_HEAD_DIM = 64
ATTN_Q_HEADS = D_MODEL // ATTN_HEAD_DIM
ATTN_KV_HEADS = 4
ATTN_GROUP = ATTN_Q_HEADS // ATTN_KV_HEADS
WINDOW = 128
ATTN_BLOCK = 128
ATTN_IN_WIDTH = (ATTN_Q_HEADS + 2 * ATTN_KV_HEADS) * ATTN_HEAD_DIM
ROPE_THETA = 10000.0

D_FF = -(-(8 * D_MODEL) // (3 * 256)) * 256
EPS = 1e-6

kernel_name = "bidir_mlstm_swa_hybrid_trunk"


def rmsnorm(x, w):
    xf = x.astype(jnp.float32)
    y = xf * lax.rsqrt(jnp.mean(xf * xf, axis=-1, keepdims=True) + EPS)
    return (y * w.astype(jnp.float32)).astype(x.dtype)


def rope(x, positions):
    half = x.shape[-1] // 2
    inv_freq = ROPE_THETA ** (-jnp.arange(half, dtype=jnp.float32) / half)
    ang = positions.astype(jnp.float32)[..., None] * inv_freq
    cos = jnp.cos(ang)[:, :, None, :]
    sin = jnp.sin(ang)[:, :, None, :]
    xf = x.astype(jnp.float32)
    x1, x2 = xf[..., :half], xf[..., half:]
    return jnp.concatenate([x1 * cos - x2 * sin, x2 * cos + x1 * sin], axis=-1)


def mlstm_chunkwise(q, k, v, log_i, log_f):
    B, H, S, dk = q.shape
    dv = v.shape[-1]
    L = MLSTM_CHUNK
    NC = S // L
    q = q.reshape(B, H, NC, L, dk)
    k = k.reshape(B, H, NC, L, dk)
    v = v.reshape(B, H, NC, L, dv)
    log_i = log_i.reshape(B, H, NC, L)
    b = jnp.cumsum(log_f.reshape(B, H, NC, L), axis=-1)
    b_last = b[..., -1]

    a = b_last[..., None] - b + log_i
    a_max = jnp.max(a, axis=-1)
    w = jnp.exp(a - a_max[..., None])
    kv_chunk = jnp.einsum('bhcl,bhcld,bhcle->bhcde', w, k, v)
    k_chunk = jnp.einsum('bhcl,bhcld->bhcd', w, k)

    def step(carry, xs):
        C, n, m = carry
        kv_c, k_c, bl_c, am_c = xs
        m_new = jnp.maximum(bl_c + m, am_c)
        s_prev = jnp.exp(bl_c + m - m_new)
        s_cur = jnp.exp(am_c - m_new)
        C_new = s_prev[..., None, None] * C + s_cur[..., None, None] * kv_c
        n_new = s_prev[..., None] * n + s_cur[..., None] * k_c
        return (C_new, n_new, m_new), (C, n, m)

    init = (jnp.zeros((B, H, dk, dv), jnp.float32),
            jnp.zeros((B, H, dk), jnp.float32),
            jnp.zeros((B, H), jnp.float32))
    xs = (jnp.moveaxis(kv_chunk, 2, 0), jnp.moveaxis(k_chunk, 2, 0),
          jnp.moveaxis(b_last, 2, 0), jnp.moveaxis(a_max, 2, 0))
    _, (C_prev, n_prev, m_prev) = lax.scan(step, init, xs)
    C_prev = jnp.moveaxis(C_prev, 0, 2)
    n_prev = jnp.moveaxis(n_prev, 0, 2)
    m_prev = jnp.moveaxis(m_prev, 0, 2)

    tril = jnp.tril(jnp.ones((L, L), dtype=bool))
    D = b[..., :, None] - b[..., None, :] + log_i[..., None, :]
    D = jnp.where(tril, D, -jnp.inf)
    g = b + m_prev[..., None]
    m_t = jnp.maximum(g, jnp.max(D, axis=-1))
    s = jnp.einsum('bhcld,bhcsd->bhcls', q, k) * jnp.exp(D - m_t[..., None])
    inter = jnp.exp(g - m_t)
    num = (jnp.einsum('bhcls,bhcse->bhcle', s, v)
           + inter[..., None] * jnp.einsum('bhcld,bhcde->bhcle', q, C_prev))
    den = jnp.sum(s, axis=-1) + inter * jnp.einsum('bhcld,bhcd->bhcl', q, n_prev)
    den = jnp.maximum(jnp.abs(den), jnp.exp(-m_t))
    return (num / den[..., None]).reshape(B, H, S, dv)


def mlstm_mixer(h, w_in, b_gate, norm_w, w_out):
    B, S, _ = h.shape
    H, dk, dv = MLSTM_HEADS, MLSTM_QK_DIM, MLSTM_V_DIM
    proj = h @ w_in
    o1 = H * dk
    o2 = 2 * H * dk
    o3 = o2 + D_MODEL
    o4 = o3 + D_MODEL
    q = proj[..., :o1].reshape(B, S, H, dk).transpose(0, 2, 1, 3).astype(jnp.float32) * (dk ** -0.5)
    k = proj[..., o1:o2].reshape(B, S, H, dk).transpose(0, 2, 1, 3).astype(jnp.float32)
    v = proj[..., o2:o3].reshape(B, S, H, dv).transpose(0, 2, 1, 3).astype(jnp.float32)
    o_gate = jax.nn.sigmoid(proj[..., o3:o4].astype(jnp.float32))
    gates = (proj[..., o4:] + b_gate).astype(jnp.float32)
    gates = gates.reshape(B, S, 4, H).transpose(2, 0, 3, 1)
    i_fwd, f_fwd, i_bwd, f_bwd = gates[0], gates[1], gates[2], gates[3]

    h_fwd = mlstm_chunkwise(q, k, v, i_fwd, jax.nn.log_sigmoid(f_fwd))
    flip = lambda t: t[:, :, ::-1]
    h_bwd = flip(mlstm_chunkwise(flip(q), flip(k), flip(v), flip(i_bwd),
                                 flip(jax.nn.log_sigmoid(f_bwd))))
    hs = h_fwd + h_bwd
    hs = hs * lax.rsqrt(jnp.mean(hs * hs, axis=-1, keepdims=True) + EPS)
    hs = hs.transpose(0, 2, 1, 3).reshape(B, S, H * dv) * norm_w.astype(jnp.float32)
    return (hs * o_gate).astype(h.dtype) @ w_out


def window_attn_mixer(h, positions, w_in, sink, w_out):
    B, S, _ = h.shape
    Hq, Hkv, G, hd, L = ATTN_Q_HEADS, ATTN_KV_HEADS, ATTN_GROUP, ATTN_HEAD_DIM, ATTN_BLOCK
    NB = S // L
    proj = h @ w_in
    q = proj[..., :Hq * hd].reshape(B, S, Hq, hd)
    k = proj[..., Hq * hd:(Hq + Hkv) * hd].reshape(B, S, Hkv, hd)
    v = proj[..., (Hq + Hkv) * hd:].reshape(B, S, Hkv, hd)
    q = rope(q, positions).reshape(B, NB, L, Hkv, G, hd)
    k = rope(k, positions)

    def band(t):
        tp = jnp.pad(t, ((0, 0), (L, L), (0, 0), (0, 0))).reshape(B, NB + 2, L, Hkv, hd)
        return jnp.concatenate([tp[:, :-2], tp[:, 1:-1], tp[:, 2:]], axis=2)

    k_band = band(k)
    v_band = band(v).astype(jnp.float32)
    scores = jnp.einsum('bcqhgd,bckhd->bchgqk', q, k_band) * (hd ** -0.5)

    qi = jnp.arange(L)
    kj = jnp.arange(3 * L)
    in_window = jnp.abs(kj[None, :] - L - qi[:, None]) <= WINDOW
    k_pos = jnp.arange(NB)[:, None] * L - L + kj[None, :]
    k_valid = (k_pos >= 0) & (k_pos < S)
    mask = in_window[None, :, :] & k_valid[:, None, :]
    scores = jnp.where(mask[None, :, None, None], scores, -jnp.inf)

    sink_l = sink.astype(jnp.float32).reshape(Hkv, G)[None, None, :, :, None]
    m = jnp.maximum(jnp.max(scores, axis=-1), sink_l)
    p = jnp.exp(scores - m[..., None])
    denom = jnp.sum(p, axis=-1) + jnp.exp(sink_l - m)
    out = jnp.einsum('bchgqk,bckhd->bcqhgd', p, v_band)
    out = out / denom.transpose(0, 1, 4, 2, 3)[..., None]
    return out.reshape(B, S, Hq * hd).astype(h.dtype) @ w_out


def swiglu(h, w_in, w_out):
    gu = h @ w_in
    gate, up = gu[..., :D_FF], gu[..., D_FF:]
    return (jax.nn.silu(gate) * up) @ w_out


def setup_inputs(seed: int = 0) -> dict:
    key = jax.random.key(seed)
    ks = jax.random.split(key, 16)
    f32 = jnp.float32

    def w(k, shape, fan_in):
        return jax.random.normal(k, shape, f32) * (fan_in ** -0.5)

    x = jax.random.normal(ks[0], (BATCH, SEQ, D_MODEL), f32)
    positions = jnp.broadcast_to(jnp.arange(SEQ, dtype=jnp.int32), (BATCH, SEQ))
    norm_mix_w = 1.0 + 0.02 * jax.random.normal(ks[1], (DEPTH, D_MODEL), f32)
    norm_ffn_w = 1.0 + 0.02 * jax.random.normal(ks[2], (DEPTH, D_MODEL), f32)
    norm_final_w = 1.0 + 0.02 * jax.random.normal(ks[3], (D_MODEL,), f32)

    mlstm_w_in = w(ks[4], (N_MLSTM_LAYERS, D_MODEL, MLSTM_IN_WIDTH), D_MODEL)
    fbias = jnp.linspace(FGATE_BIAS_LO, FGATE_BIAS_HI, MLSTM_HEADS, dtype=f32)
    base = jnp.stack([jnp.zeros_like(fbias), fbias, jnp.zeros_like(fbias), fbias], axis=0)
    mlstm_b_gate = (base[None] + 0.1 * jax.random.normal(ks[5], (N_MLSTM_LAYERS, 4, MLSTM_HEADS), f32)
                    ).reshape(N_MLSTM_LAYERS, MLSTM_N_GATES)
    mlstm_norm_w = 1.0 + 0.02 * jax.random.normal(ks[6], (N_MLSTM_LAYERS, D_MODEL), f32)
    mlstm_w_out = w(ks[7], (N_MLSTM_LAYERS, D_MODEL, D_MODEL), D_MODEL)

    attn_w_in = w(ks[8], (N_ATTN_LAYERS, D_MODEL, ATTN_IN_WIDTH), D_MODEL)
    attn_sink = 0.5 * jax.random.normal(ks[9], (N_ATTN_LAYERS, ATTN_Q_HEADS), f32)
    attn_w_out = w(ks[10], (N_ATTN_LAYERS, ATTN_Q_HEADS * ATTN_HEAD_DIM, D_MODEL), ATTN_Q_HEADS * ATTN_HEAD_DIM)

    ffn_w_in = w(ks[11], (DEPTH, D_MODEL, 2 * D_FF), D_MODEL)
    ffn_w_out = w(ks[12], (DEPTH, D_FF, D_MODEL), D_FF)
    return {"x": x, "positions": positions, "norm_mix_w": norm_mix_w, "norm_ffn_w": norm_ffn_w,
            "norm_final_w": norm_final_w, "mlstm_w_in": mlstm_w_in, "mlstm_b_gate": mlstm_b_gate,
            "mlstm_norm_w": mlstm_norm_w, "mlstm_w_out": mlstm_w_out, "attn_w_in": attn_w_in,
            "attn_sink": attn_sink, "attn_w_out": attn_w_out, "ffn_w_in": ffn_w_in,
            "ffn_w_out": ffn_w_out}


def reference(x, positions, norm_mix_w, norm_ffn_w, norm_final_w, mlstm_w_in, mlstm_b_gate,
              mlstm_norm_w, mlstm_w_out, attn_w_in, attn_sink, attn_w_out, ffn_w_in, ffn_w_out):
    for i in range(DEPTH):
        j = i // N_MIXERS
        hn = rmsnorm(x, norm_mix_w[i])
        if i % N_MIXERS == 0:
            x = x + mlstm_mixer(hn, mlstm_w_in[j], mlstm_b_gate[j], mlstm_norm_w[j], mlstm_w_out[j])
        else:
            x = x + window_attn_mixer(hn, positions, attn_w_in[j], attn_sink[j], attn_w_out[j])
        x = x + swiglu(rmsnorm(x, norm_ffn_w[i]), ffn_w_in[i], ffn_w_out[i])
    return rmsnorm(x, norm_final_w)
```

```python
import math
from types import SimpleNamespace
import numpy as np
import ml_dtypes
import concourse.bass as bass
import concourse.mybir as mybir
from concourse.bass_utils import run_bass_kernel_spmd

F32 = mybir.dt.float32
BF16 = mybir.dt.bfloat16
I32 = mybir.dt.int32
U8 = mybir.dt.uint8
ALU = mybir.AluOpType
AF = mybir.ActivationFunctionType

SEM_LIMIT = 30000
DSZ = {F32: 4, BF16: 2, I32: 4, U8: 1}

D = 1024
T = 2048
S = 8192
DFF = 2816
NJ = 22
EPS = 1e-6
PI = math.pi
TWO_PI = 2.0 * math.pi
C1 = 6.28125
C2 = TWO_PI - C1
PI_SAFE = 3.1415925


class Sem:
    __slots__ = ("h", "cnt", "name")

    def __init__(self, h, name):
        self.h = h
        self.cnt = 0
        self.name = name


class Res:
    __slots__ = ("name", "lw", "rd", "excl", "dsem")

    def __init__(self, name, excl=False):
        self.name = name
        self.lw = None
        self.rd = {}
        self.excl = excl
        self.dsem = None


class Prog:
    ENG = ("pe", "act", "dve", "pool", "sp")

    def __init__(self, nc):
        self.nc = nc
        self.streams = {e: [] for e in self.ENG}
        self.cur = {e: None for e in self.ENG}
        self.nins = {e: 0 for e in self.ENG}
        self.seen = {e: {} for e in self.ENG}
        self.nsem = 0
        self.final_waits = []
        self.dsems = []
        self.esems = []

    def newsem(self, name):
        self.nsem += 1
        return Sem(self.nc.alloc_semaphore(f"{name}_{self.nsem}"), f"{name}_{self.nsem}")

    def cursem(self, eng):
        if self.cur[eng] is None:
            self.cur[eng] = self.newsem("e" + eng)
            self.esems.append((eng, self.cur[eng]))
        return self.cur[eng]

    def _waits(self, eng, reads, writes, chain=False):
        waits = {}
        seen = self.seen[eng]

        def need(rec):
            if rec is None:
                return
            pe, s, c, idx = rec
            if pe == eng:
                if eng == "pe":
                    return
                if self.nins[eng] - idx > 2:
                    return
            if seen.get(s, 0) >= c:
                return
            if waits.get(s, 0) < c:
                waits[s] = c

        for r in reads:
            need(r.lw)
            if r.excl:
                for rec in r.rd.values():
                    need(rec)
        for w in writes:
            if not (chain and w.lw is not None and w.lw[0] == "dma"):
                need(w.lw)
            for rec in w.rd.values():
                need(rec)
        for s, c in waits.items():
            seen[s] = c
        return list(waits.items())

    def _record(self, rec, reads, writes):
        for r in reads:
            if r.excl:
                r.lw = rec
                r.rd = {}
            else:
                r.rd[(rec[0], rec[1])] = rec
        for w in writes:
            w.lw = rec
            w.rd = {}

    def emit(self, eng, fn, reads=(), writes=(), inc=True):
        waits = self._waits(eng, reads, writes)
        S_ = self.cursem(eng)
        if inc:
            S_.cnt += 1
            rec = (eng, S_, S_.cnt, self.nins[eng])
        else:
            rec = (eng, S_, S_.cnt + 1, self.nins[eng])
        self._record(rec, reads, writes)
        self.streams[eng].append((waits, fn, (S_, 1) if inc else None))
        self.nins[eng] += 1
        if inc and S_.cnt >= SEM_LIMIT:
            self.cur[eng] = None

    def dma(self, q, out, in_, reads=(), writes=(), semres=None, chain=False, **kw):
        waits = self._waits(q, reads, writes, chain=chain)
        sr = semres if semres is not None else (writes[0] if writes else reads[0])
        if sr.dsem is None:
            sr.dsem = self.newsem("d")
            self.dsems.append(sr.dsem)
        Dm = sr.dsem
        Dm.cnt += 16
        rec = ("dma", Dm, Dm.cnt, -1)
        self._record(rec, reads, writes)
        self.streams[q].append((waits, lambda e: e.dma_start(out=out, in_=in_, **kw), (Dm, 16)))
        self.nins[q] += 1
        return rec

    def barrier(self):
        allw = [(s, s.cnt) for _, s in self.esems if s.cnt > 0] + [(d, d.cnt) for d in self.dsems if d.cnt > 0]
        for eng in self.ENG:
            w = []
            for s, c in allw:
                if self.seen[eng].get(s, 0) < c:
                    w.append((s, c))
                    self.seen[eng][s] = c
            if w:
                self.streams[eng].append((w, None, None))

    def build(self):
        nc = self.nc
        fw = {}
        for rec in self.final_waits:
            _, s, c, _ = rec
            fw[s] = max(fw.get(s, 0), c)
        streams = self.streams

        def replay(eng, e):
            for waits, fn, inc in streams[eng]:
                for s, c in waits:
                    e.wait_ge(s.h, c)
                if fn is None:
                    continue
                ins = fn(e)
                if inc is not None:
                    ins.then_inc(inc[0].h, inc[1])
            if eng == "sp":
                for s, c in fw.items():
                    e.wait_ge(s.h, c)

        with nc.Block() as block:
            @block.tensor
            def _(e):
                replay("pe", e)

            @block.scalar
            def _(e):
                replay("act", e)

            @block.vector
            def _(e):
                replay("dve", e)

            @block.gpsimd
            def _(e):
                replay("pool", e)

            @block.sync
            def _(e):
                replay("sp", e)


class Arena:
    def __init__(self, big, base, limit):
        self.big = big
        self.base = base
        self.cur = base
        self.limit = limit

    def reset(self):
        self.cur = self.base

    def alloc(self, shape, dtype, parts=128):
        n = int(np.prod(shape[1:])) * DSZ[dtype]
        off = self.cur
        self.cur += (n + 63) // 64 * 64
        assert self.cur <= self.limit, ("SBUF arena overflow", self.cur, self.limit)
        ap = self.big[0:shape[0], off:off + n].bitcast(dtype)
        if len(shape) == 3:
            ap = ap.rearrange("p (a b) -> p a b", b=shape[2])
        elif len(shape) == 4:
            ap = ap.rearrange("p (a b c) -> p a b c", b=shape[2], c=shape[3])
        return ap


CST_W = 784


def make_consts():
    c = np.zeros((128, CST_W), np.float32)
    i = np.arange(128)
    c[:, 0:128] = np.eye(128, dtype=np.float32)
    c[:, 128:256] = (i[:, None] <= i[None, :]).astype(np.float32)
    c[:, 256:384] = (i[:, None] >= i[None, :]).astype(np.float32)
    r = np.arange(384)
    c[:, 384:768] = (np.abs(r[None, :] - i[:, None] - 128) <= 128).astype(np.float32)
    d = i % 64
    c[:, 768] = (10000.0 ** (-(d % 32).astype(np.float32) / np.float32(32))).astype(np.float32)
    c[:, 769] = np.where(d < 32, -1.0, 1.0)
    c[:, 770] = EPS
    c[:, 771] = 1.0
    return c


class Ctx:
    def __init__(self, nc, cst_dram):
        self.nc = nc
        self.P = Prog(nc)
        avail = (nc.sbuf_bytes_remaining - 2048) // 256 * 256
        self.big = nc.alloc_sbuf_tensor("big", [128, avail], U8)
        ca = Arena(self.big, 0, avail)
        self.cstf = ca.alloc([128, CST_W], F32)
        self.ident = ca.alloc([128, 128], BF16)
        self.ones_bf = ca.alloc([128, 128], BF16)
        self.ones_f = ca.alloc([128, 128], F32)
        self.band_bf = ca.alloc([128, 384], BF16)
        self.A = Arena(self.big, ca.cur, avail)
        self.ps = [nc.alloc_psum_tensor(f"ps{i}", [128, 512], F32) for i in range(8)]
        self.rps = [Res(f"ps{i}", excl=True) for i in range(8)]
        self.r_cst = Res("cst")
        P = self.P
        P.dma("sp", self.cstf, cst_dram, writes=[self.r_cst])
        P.emit("dve", lambda e: e.tensor_copy(self.ident, self.cstf[:, 0:128]), reads=[self.r_cst], writes=[self.r_cst])
        P.emit("dve", lambda e: e.memset(self.ones_bf, 1.0), writes=[self.r_cst])
        P.emit("dve", lambda e: e.memset(self.ones_f, 1.0), writes=[self.r_cst])
        P.emit("dve", lambda e: e.tensor_copy(self.band_bf, self.cstf[:, 384:768]), reads=[self.r_cst], writes=[self.r_cst])
        self.triL = self.cstf[:, 128:256]
        self.triU = self.cstf[:, 256:384]
        self.invf = self.cstf[:, 768:769]
        self.sgn = self.cstf[:, 769:770]
        self.epsc = self.cstf[:, 770:771]


def tp_alloc(G):
    A = G.A
    c = SimpleNamespace()
    c.xT = A.alloc([128, 8, T], F32)
    c.hn = A.alloc([128, 8, 1024], BF16)
    c.act = A.alloc([128, NJ, 1024], BF16)
    c.w2 = A.alloc([128, NJ, 1024], BF16)
    c.w1 = [A.alloc([128, 8, 2, 128], BF16) for _ in range(4)]
    c.sq = A.alloc([128, 8, 512], BF16)
    c.ln = A.alloc([128, 512], F32)
    c.rstd = A.alloc([128, 512], F32)
    c.sg = [A.alloc([128, 512], BF16) for _ in range(2)]
    c.ncol = A.alloc([128, 3, 8], F32)
    ov = Arena(G.big, 0, 0)
    base = c.act
    c.r_x = [Res(f"x{t}") for t in range(4)]
    c.r_hn = [Res("hn0"), Res("hn1")]
    c.r_act = [Res(f"act{t}") for t in range(2)]
    c.r_w2 = Res("w2")
    c.r_w1 = [Res(f"w1_{i}") for i in range(4)]
    c.r_sq = Res("sq")
    c.r_ln = Res("ln")
    c.r_rstd = Res("rstd")
    c.r_sg = [Res("sg0"), Res("sg1")]
    c.r_ncol = Res("ncol")
    c.r_ov = [Res("ovm0"), Res("ovm1"), Res("ovw"), Res("ovy0"), Res("ovy1")]
    return c


def tp_overlay(G, c):
    big = G.big
    return None


def emit_norm_tg(G, c, tg, wcol, out_fn, r_out, ps_i):
    P = G.P
    sl = slice(tg * 512, (tg + 1) * 512)
    P.emit("act", lambda e: e.activation(out=c.sq, in_=c.xT[:, :, sl], func=AF.Square),
           reads=[c.r_x[tg]], writes=[c.r_sq])
    ps = G.ps[ps_i]
    for ch in range(8):
        P.emit("pe", lambda e, ch=ch: e.matmul(ps[:, :], lhsT=G.ones_bf, rhs=c.sq[:, ch, :], start=(ch == 0), stop=(ch == 7)),
               reads=[c.r_sq, G.r_cst], writes=[G.rps[ps_i]], inc=(ch == 7))
    P.emit("act", lambda e: e.activation(out=c.ln, in_=ps[:, :], func=AF.Ln, scale=1.0 / D, bias=G.epsc),
           reads=[G.rps[ps_i], G.r_cst], writes=[c.r_ln])
    P.emit("act", lambda e: e.activation(out=c.rstd, in_=c.ln, func=AF.Exp, scale=-0.5),
           reads=[c.r_ln], writes=[c.r_rstd])
    for ch in range(8):
        P.emit("dve", lambda e, ch=ch: e.scalar_tensor_tensor(out=out_fn(ch), in0=c.xT[:, ch, sl], scalar=wcol[:, ch:ch + 1],
                                                               in1=c.rstd, op0=ALU.mult, op1=ALU.mult),
               reads=[c.r_x[tg], c.r_ncol, c.r_rstd], writes=[r_out])


def emit_ffn(G, c, w_in, w_out, wcol):
    P = G.P
    w_in_v = w_in.rearrange("(c p) (g f) -> p c g f", p=128, g=2)
    w_out_v = w_out.rearrange("(j p) d -> p j d", p=128)
    for q4 in range(4):
        j0, j1 = q4 * 6, min(NJ, q4 * 6 + 6)
        P.dma("pool", c.w2[:, j0:j1, :], w_out_v[:, j0:j1, :], writes=[c.r_w2], chain=(q4 > 0))
    nload = [0]

    def load_w1(j):
        b = nload[0] % 4
        nload[0] += 1
        P.dma("pool", c.w1[b][:, :, 0, :], w_in_v[:, :, 0, j * 128:(j + 1) * 128], writes=[c.r_w1[b]])
        P.dma("pool", c.w1[b][:, :, 1, :], w_in_v[:, :, 1, j * 128:(j + 1) * 128], writes=[c.r_w1[b]], chain=True)
        return b

    for half in range(2):
        for tl in range(2):
            tg = half * 2 + tl
            emit_norm_tg(G, c, tg, wcol, lambda ch, tl=tl: c.hn[:, ch, tl * 512:(tl + 1) * 512], c.r_hn[tl], 7)
        pend = [load_w1(0), load_w1(1)]
        k = 0
        for j in range(NJ):
            b = pend.pop(0)
            if j + 2 < NJ:
                pend.append(load_w1(j + 2))
            for tl in range(2):
                pg, pu = (k % 2) * 2, (k % 2) * 2 + 1
                k += 1
                for g_, pi in ((0, pg), (1, pu)):
                    for ch in range(8):
                        P.emit("pe", lambda e, ch=ch, g_=g_, pi=pi, b=b, tl=tl: e.matmul(
                            G.ps[pi][:, :], lhsT=c.w1[b][:, ch, g_, :], rhs=c.hn[:, ch, tl * 512:(tl + 1) * 512],
                            start=(ch == 0), stop=(ch == 7)),
                            reads=[c.r_w1[b], c.r_hn[tl]], writes=[G.rps[pi]], inc=(ch == 7))
                sgi = k % 2
                P.emit("act", lambda e, pg=pg, sgi=sgi: e.activation(out=c.sg[sgi], in_=G.ps[pg][:, :], func=AF.Silu),
                       reads=[G.rps[pg]], writes=[c.r_sg[sgi]])
                P.emit("dve", lambda e, pu=pu, sgi=sgi, j=j, tl=tl: e.tensor_tensor(
                    out=c.act[:, j, tl * 512:(tl + 1) * 512], in0=G.ps[pu][:, :], in1=c.sg[sgi], op=ALU.mult),
                    reads=[G.rps[pu], c.r_sg[sgi]], writes=[c.r_act[tl]])
        k2 = 0
        for tl in range(2):
            tg = half * 2 + tl
            for dt in range(8):
                pi = 4 + (k2 % 3)
                k2 += 1
                for j in range(NJ):
                    P.emit("pe", lambda e, j=j, dt=dt, pi=pi, tl=tl: e.matmul(
                        G.ps[pi][:, :], lhsT=c.w2[:, j, dt * 128:(dt + 1) * 128], rhs=c.act[:, j, tl * 512:(tl + 1) * 512],
                        start=(j == 0), stop=(j == NJ - 1)),
                        reads=[c.r_w2, c.r_act[tl]], writes=[G.rps[pi]], inc=(j == NJ - 1))
                P.emit("dve", lambda e, dt=dt, pi=pi, tg=tg: e.tensor_tensor(
                    out=c.xT[:, dt, tg * 512:(tg + 1) * 512], in0=G.ps[pi][:, :], in1=c.xT[:, dt, tg * 512:(tg + 1) * 512], op=ALU.add),
                    reads=[G.rps[pi], c.r_x[tg]], writes=[c.r_x[tg]])


def emit_wout(G, c, mT, w_out, ov):
    P = G.P
    mT_v = mT.rearrange("(c p) t -> p c t", p=128)
    wv = w_out.rearrange("(c p) d -> p c d", p=128)
    P.dma("pool", ov.wo[:, 0:4], wv[:, 0:4], writes=[c.r_ov[2]])
    P.dma("pool", ov.wo[:, 4:8], wv[:, 4:8], writes=[c.r_ov[2]], chain=True)
    k = 0
    for tg in range(4):
        b = tg % 2
        P.dma("sp", ov.m[b], mT_v[:, :, tg * 512:(tg + 1) * 512], writes=[c.r_ov[b]])
        for dt in range(8):
            pi = k % 4
            k += 1
            for ch in range(8):
                P.emit("pe", lambda e, ch=ch, dt=dt, pi=pi, b=b: e.matmul(
                    G.ps[pi][:, :], lhsT=ov.wo[:, ch, dt * 128:(dt + 1) * 128], rhs=ov.m[b][:, ch, :],
                    start=(ch == 0), stop=(ch == 7)),
                    reads=[c.r_ov[2], c.r_ov[b]], writes=[G.rps[pi]], inc=(ch == 7))
            P.emit("dve", lambda e, dt=dt, pi=pi, tg=tg: e.tensor_tensor(
                out=c.xT[:, dt, tg * 512:(tg + 1) * 512], in0=G.ps[pi][:, :], in1=c.xT[:, dt, tg * 512:(tg + 1) * 512], op=ALU.add),
                reads=[G.rps[pi], c.r_x[tg]], writes=[c.r_x[tg]])


def tp_make_overlay(G, c):
    ov = SimpleNamespace()
    a = Arena(G.big, c.act_off, c.act_off + 45056)
    ov.m = [a.alloc([128, 8, 512], BF16) for _ in range(2)]
    ov.wo = a.alloc([128, 8, 1024], BF16)
    a2 = Arena(G.big, c.act_off, c.act_off + 45056)
    ov.y = [a2.alloc([128, 8, 512], F32) for _ in range(2)]
    return ov


def build_tp(has_mix, has_ffn, final):
    nc = bass.Bass("TRN2", target_bir_lowering=False)
    cst = nc.dram_tensor("cst", [128, CST_W], F32, kind="ExternalInput").ap()
    xT = nc.dram_tensor("xT", [D, T], F32, kind="ExternalInput").ap()
    ncol = nc.dram_tensor("ncol", [128, 3, 8], F32, kind="ExternalInput").ap()
    if has_mix:
        mT = nc.dram_tensor("mT", [D, T], BF16, kind="ExternalInput").ap()
        wo = nc.dram_tensor("wo", [D, D], F32, kind="ExternalInput").ap()
    if has_ffn:
        w1 = nc.dram_tensor("w1", [D, 2 * DFF], F32, kind="ExternalInput").ap()
        w2 = nc.dram_tensor("w2", [DFF, D], F32, kind="ExternalInput").ap()
    if final:
        yT = nc.dram_tensor("yT", [D, T], F32, kind="ExternalOutput").ap()
    else:
        xo = nc.dram_tensor("xo", [D, T], F32, kind="ExternalOutput").ap()
        hnT = nc.dram_tensor("hnT", [D, T], BF16, kind="ExternalOutput").ap()
    G = Ctx(nc, cst)
    P = G.P
    off0 = G.A.cur
    c = tp_alloc(G)
    c.act_off = off0 + 8 * T * 4 + 8 * 1024 * 2
    ov = tp_make_overlay(G, c)
    xv = xT.rearrange("(c p) t -> p c t", p=128)
    for tg in range(4):
        P.dma("sp", c.xT[:, :, tg * 512:(tg + 1) * 512], xv[:, :, tg * 512:(tg + 1) * 512], writes=[c.r_x[tg]])
    P.dma("sp", c.ncol, ncol, writes=[c.r_ncol])
    if has_mix:
        emit_wout(G, c, mT, wo, ov)
        P.barrier()
    if has_ffn:
        emit_ffn(G, c, w1, w2, c.ncol[:, 0, :])
        P.barrier()
    if final:
        yv = yT.rearrange("(c p) t -> p c t", p=128)
        for tg in range(4):
            b = tg % 2
            emit_norm_tg(G, c, tg, c.ncol[:, 1, :], lambda ch, b=b: ov.y[b][:, ch, :], c.r_ov[3 + b], 7)
            rec = P.dma("sp", yv[:, :, tg * 512:(tg + 1) * 512], ov.y[b], reads=[c.r_ov[3 + b]])
            P.final_waits.append(rec)
    else:
        xov = xo.rearrange("(c p) t -> p c t", p=128)
        hv = hnT.rearrange("(c p) t -> p c t", p=128)
        for tg in range(4):
            rec = P.dma("sp", xov[:, :, tg * 512:(tg + 1) * 512], c.xT[:, :, tg * 512:(tg + 1) * 512], reads=[c.r_x[tg]])
            P.final_waits.append(rec)
        for tg in range(4):
            hb, tl = tg // 2, tg % 2
            emit_norm_tg(G, c, tg, c.ncol[:, 1, :], lambda ch, tl=tl: c.hn[:, ch, tl * 512:(tl + 1) * 512], c.r_hn[tl], 7)
            rec = P.dma("sp", hv[:, :, tg * 512:(tg + 1) * 512], c.hn[:, :, tl * 512:(tl + 1) * 512], reads=[c.r_hn[tl]])
            P.final_waits.append(rec)
    P.build()
    return nc


_PROGS = {}


def _prog(key, fn):
    if key not in _PROGS:
        _PROGS[key] = fn()
    return _PROGS[key]


def _cols(w):
    return np.ascontiguousarray(np.asarray(w, np.float32).reshape(8, 128).T)


def run_tp(xT_sh, mT_sh, wo, w1, w2, ncol_ffn, ncol_next, final):
    has_mix = mT_sh is not None
    has_ffn = w1 is not None
    nc = _prog(("tp", has_mix, has_ffn, final), lambda: build_tp(has_mix, has_ffn, final))
    cst = make_consts()
    maps = []
    for r in range(8):
        nco = np.zeros((128, 3, 8), np.float32)
        if ncol_ffn is not None:
            nco[:, 0, :] = _cols(ncol_ffn)
        nco[:, 1, :] = _cols(ncol_next)
        m = {"cst": cst, "xT": xT_sh[r], "ncol": nco}
        if has_mix:
            m["mT"] = mT_sh[r]
            m["wo"] = wo
        if has_ffn:
            m["w1"] = w1
            m["w2"] = w2
        maps.append(m)
    res = run_bass_kernel_spmd(nc, maps, core_ids=list(range(8)))
    if final:
        return [r_["yT"] for r_ in res.results]
    return [r_["xo"] for r_ in res.results], [r_["hnT"] for r_ in res.results]


def build_mlstm():
    nc = bass.Bass("TRN2", target_bir_lowering=False)
    cst = nc.dram_tensor("cst", [128, CST_W], F32, kind="ExternalInput").ap()
    hnT = nc.dram_tensor("hnT", [4 * D, T], BF16, kind="ExternalInput").ap()
    wfm = nc.dram_tensor("wfm", [D, 512], F32, kind="ExternalInput").ap()
    wtk = nc.dram_tensor("wtk", [D, 388], F32, kind="ExternalInput").ap()
    bg = nc.dram_tensor("bg", [1, 4], F32, kind="ExternalInput").ap()
    nw = nc.dram_tensor("nw", [128, 2], F32, kind="ExternalInput").ap()
    outT = nc.dram_tensor("outT", [4 * 256, T], BF16, kind="ExternalOutput").ap()
    G = Ctx(nc, cst)
    emit_mlstm(G, hnT, wfm, wtk, bg, nw, outT, final=True)
    G.P.build()
    return nc


def emit_mlstm(G, hnT, wfm, wtk, bg, nw, outT, final=False):
    P, A = G.P, G.A
    NCH = 64
    qT = A.alloc([128, S], BF16)
    kT = A.alloc([128, S], BF16)
    ktok = A.alloc([128, NCH, 128], BF16)
    vext = A.alloc([128, NCH, 257], BF16)
    sigo = A.alloc([128, 2, S], BF16)
    Cb = A.alloc([128, NCH, 257], BF16)
    Gt = A.alloc([128, NCH, 4], F32)
    Lt = A.alloc([128, 2, NCH], F32)
    Et = A.alloc([128, 2, NCH], F32)
    W6 = A.alloc([128, 6, NCH], F32)
    EE = A.alloc([128, NCH, 2], F32)
    hnb = [A.alloc([128, 8, 512], BF16) for _ in range(2)]
    wfm_s = A.alloc([128, 8, 512], BF16)
    wtk_s = A.alloc([128, 8, 388], BF16)
    bgs = A.alloc([128, 4], F32)
    nws = A.alloc([128, 2], F32)
    U = [A.alloc([128, 257], F32) for _ in range(2)]
    Cf = A.alloc([128, 257], BF16)
    Vw = [A.alloc([128, 257], BF16) for _ in range(2)]
    Sm = [A.alloc([128, 128], BF16) for _ in range(2)]
    hh = A.alloc([128, 256], F32)
    junk = A.alloc([128, 256], F32)
    hbf = A.alloc([128, 256], BF16)
    dn = A.alloc([128, 8], F32)
    stage = [A.alloc([128, 2, 512], BF16) for _ in range(2)]
    R = lambda n, **k: Res(n, **k)
    r_q, r_k, r_kt, r_v, r_so, r_cb, r_g, r_l, r_e, r_w6, r_ee = [R(n) for n in "q k kt v so cb g l e w6 ee".split()]
    r_hnb = [R("hnb0"), R("hnb1")]
    r_w, r_bg, r_nw = R("w"), R("bg"), R("nw")
    r_U = [R("Ub"), R("Uf")]
    r_cf = R("cf")
    r_vw = [R("vwf"), R("vwb")]
    r_sm = [R("smf"), R("smb")]
    r_h, r_junk, r_hbf, r_dn = R("h"), R("junk"), R("hbf"), R("dn")
    r_st = [R("st0"), R("st1")]
    ps, rps = G.ps, G.rps
    ps_bf = [p_[:, :].bitcast(BF16) for p_ in ps]

    wf_v = wfm.rearrange("(c p) f -> p c f", p=128)
    wt_v = wtk.rearrange("(c p) f -> p c f", p=128)
    P.dma("pool", wfm_s[:, 0:4], wf_v[:, 0:4], writes=[r_w])
    P.dma("pool", wfm_s[:, 4:8], wf_v[:, 4:8], writes=[r_w], chain=True)
    P.dma("pool", wtk_s, wt_v, writes=[r_w], chain=True)
    P.dma("sp", bgs, bg[0:1, :].partition_broadcast(128), writes=[r_bg])
    P.dma("sp", nws, nw, writes=[r_nw])
    P.emit("pool", lambda e: e.memset(vext[:, :, 256:257], 1.0), writes=[r_v])

    k = 0
    for tg in range(16):
        b = tg % 2
        rr, tl0 = tg // 4, (tg % 4) * 512
        src = hnT[rr * D:(rr + 1) * D, tl0:tl0 + 512].rearrange("(c p) t -> p c t", p=128)
        P.dma("sp", hnb[b], src, writes=[r_hnb[b]])
        tsl = slice(tg * 512, (tg + 1) * 512)
        for ft in range(4):
            pi = k % 4
            k += 1
            for ch in range(8):
                P.emit("pe", lambda e, ch=ch, ft=ft, pi=pi, b=b: e.matmul(
                    ps[pi][:, :], lhsT=wfm_s[:, ch, ft * 128:(ft + 1) * 128], rhs=hnb[b][:, ch, :], start=(ch == 0), stop=(ch == 7)),
                    reads=[r_w, r_hnb[b]], writes=[rps[pi]], inc=(ch == 7))
            if ft == 0:
                P.emit("act", lambda e, pi=pi, tsl=tsl: e.activation(out=qT[:, tsl], in_=ps[pi][:, :], func=AF.Copy, scale=128.0 ** -0.5),
                       reads=[rps[pi]], writes=[r_q])
            elif ft == 1:
                P.emit("dve", lambda e, pi=pi, tsl=tsl: e.tensor_copy(kT[:, tsl], ps[pi][:, :]), reads=[rps[pi]], writes=[r_k])
            else:
                P.emit("act", lambda e, pi=pi, tsl=tsl, ft=ft: e.activation(out=sigo[:, ft - 2, tsl], in_=ps[pi][:, :], func=AF.Sigmoid),
                       reads=[rps[pi]], writes=[r_so])
        for cl in range(4):
            c_ = tg * 4 + cl
            pi = 4 + (c_ % 2)
            for ch in range(8):
                P.emit("pe", lambda e, ch=ch, cl=cl, pi=pi, b=b: e.matmul(
                    ps[pi][:, 0:388], lhsT=hnb[b][:, ch, cl * 128:(cl + 1) * 128], rhs=wtk_s[:, ch, :], start=(ch == 0), stop=(ch == 7)),
                    reads=[r_w, r_hnb[b]], writes=[rps[pi]], inc=(ch == 7))
            P.emit("act", lambda e, pi=pi, c_=c_: e.activation(out=ktok[:, c_, :], in_=ps[pi][:, 0:128], func=AF.Copy),
                   reads=[rps[pi]], writes=[r_kt])
            P.emit("dve", lambda e, pi=pi, c_=c_: e.tensor_copy(vext[:, c_, 0:256], ps[pi][:, 128:384]), reads=[rps[pi]], writes=[r_v])
            P.emit("dve", lambda e, pi=pi, c_=c_: e.tensor_tensor(out=Gt[:, c_, :], in0=ps[pi][:, 384:388], in1=bgs, op=ALU.add),
                   reads=[rps[pi], r_bg], writes=[r_g])

    for d_, col in ((0, 1), (1, 3)):
        P.emit("act", lambda e, d_=d_, col=col: e.activation(out=Et[:, d_, :], in_=Gt[:, :, col], func=AF.Exp, scale=-1.0),
               reads=[r_g], writes=[r_e])
        P.emit("act", lambda e, d_=d_: e.activation(out=Lt[:, d_, :], in_=Et[:, d_, :], func=AF.Ln, bias=1.0),
               reads=[r_e], writes=[r_l])
    Lflat = Lt.rearrange("p a b -> p (a b)")
    P.emit("pe", lambda e: e.matmul(ps[0][:, 0:128], lhsT=G.triL, rhs=Lflat, start=True, stop=True), reads=[r_l, G.r_cst], writes=[rps[0]])
    P.emit("pe", lambda e: e.matmul(ps[1][:, 0:128], lhsT=G.triU, rhs=Lflat, start=True, stop=True), reads=[r_l, G.r_cst], writes=[rps[1]])
    P.emit("pe", lambda e: e.matmul(ps[2][:, 0:128], lhsT=G.ones_f, rhs=Lflat, start=True, stop=True), reads=[r_l, G.r_cst], writes=[rps[2]])
    for d_, (pc, lo, icol) in enumerate(((0, 0, 0), (1, 64, 2))):
        P.emit("dve", lambda e, d_=d_, pc=pc, lo=lo, icol=icol: e.tensor_tensor(out=W6[:, 4 + d_, :], in0=ps[pc][:, lo:lo + 64], in1=Gt[:, :, icol], op=ALU.add),
               reads=[rps[pc], r_g], writes=[r_w6])
        P.emit("act", lambda e, d_=d_: e.activation(out=W6[:, d_, :], in_=W6[:, 4 + d_, :], func=AF.Exp), reads=[r_w6], writes=[r_w6])
        P.emit("act", lambda e, d_=d_, pc=pc, lo=lo: e.activation(out=EE[:, :, d_], in_=ps[pc][:, lo:lo + 64], func=AF.Exp, scale=-1.0),
               reads=[rps[pc]], writes=[r_ee])
        P.emit("act", lambda e, d_=d_, lo=lo: e.activation(out=W6[:, 2 + d_, :], in_=ps[2][:, lo:lo + 64], func=AF.Exp, scale=-1.0),
               reads=[rps[2]], writes=[r_w6])
    wF, wB, decF, decB = (lambda c_: W6[:, 0, c_:c_ + 1]), (lambda c_: W6[:, 1, c_:c_ + 1]), (lambda c_: W6[:, 2, c_:c_ + 1]), (lambda c_: W6[:, 3, c_:c_ + 1])

    P.emit("pool", lambda e: e.memset(Cb[:, NCH - 1, :], 0.0), writes=[r_cb])
    for c_ in range(NCH - 1, 0, -1):
        pi = 6 + (c_ % 2)
        P.emit("act", lambda e, c_=c_: e.activation(out=Vw[1], in_=vext[:, c_, :], func=AF.Copy, scale=wB(c_)), reads=[r_v, r_w6], writes=[r_vw[1]])
        P.emit("pe", lambda e, c_=c_, pi=pi: e.matmul(ps[pi][:, 0:257], lhsT=ktok[:, c_, :], rhs=Vw[1], start=True, stop=True),
               reads=[r_kt, r_vw[1]], writes=[rps[pi]])
        if c_ == NCH - 1:
            P.emit("dve", lambda e, pi=pi: e.tensor_copy(U[0], ps[pi][:, 0:257]), reads=[rps[pi]], writes=[r_U[0]])
        else:
            P.emit("dve", lambda e, c_=c_, pi=pi: e.scalar_tensor_tensor(out=U[0], in0=U[0], scalar=decB(c_ + 1), in1=ps[pi][:, 0:257], op0=ALU.mult, op1=ALU.add),
                   reads=[r_U[0], r_w6, rps[pi]], writes=[r_U[0]])
        P.emit("act", lambda e, c_=c_: e.activation(out=Cb[:, c_ - 1, :], in_=U[0], func=AF.Copy, scale=decB(c_)), reads=[r_U[0], r_w6], writes=[r_cb])

    for c_ in range(NCH):
        csl = slice(c_ * 128, (c_ + 1) * 128)
        P.emit("pe", lambda e, csl=csl: e.matmul(ps[0][:, 0:128], lhsT=kT[:, csl], rhs=qT[:, csl], start=True, stop=True),
               reads=[r_k, r_q], writes=[rps[0]])
        P.emit("dve", lambda e: e.tensor_tensor(out=Sm[0], in0=ps[0][:, 0:128], in1=G.triL, op=ALU.mult), reads=[rps[0], G.r_cst], writes=[r_sm[0]])
        P.emit("dve", lambda e: e.tensor_tensor(out=Sm[1], in0=ps[0][:, 0:128], in1=G.triU, op=ALU.mult), reads=[rps[0], G.r_cst], writes=[r_sm[1]])
        P.emit("act", lambda e, c_=c_: e.activation(out=Vw[0], in_=vext[:, c_, :], func=AF.Copy, scale=wF(c_)), reads=[r_v, r_w6], writes=[r_vw[0]])
        P.emit("act", lambda e, c_=c_: e.activation(out=Vw[1], in_=vext[:, c_, :], func=AF.Copy, scale=wB(c_)), reads=[r_v, r_w6], writes=[r_vw[1]])
        lastf = (c_ == 0)
        P.emit("pe", lambda e, lastf=lastf: e.matmul(ps[1][:, 0:257], lhsT=Sm[0], rhs=Vw[0], start=True, stop=lastf),
               reads=[r_sm[0], r_vw[0]], writes=[rps[1]], inc=lastf)
        if not lastf:
            P.emit("pe", lambda e, csl=csl: e.matmul(ps[1][:, 0:257], lhsT=qT[:, csl], rhs=Cf, start=False, stop=True),
                   reads=[r_q, r_cf], writes=[rps[1]])
        lastb = (c_ == NCH - 1)
        P.emit("pe", lambda e, lastb=lastb: e.matmul(ps[2][:, 0:257], lhsT=Sm[1], rhs=Vw[1], start=True, stop=lastb),
               reads=[r_sm[1], r_vw[1]], writes=[rps[2]], inc=lastb)
        if not lastb:
            P.emit("pe", lambda e, csl=csl, c_=c_: e.matmul(ps[2][:, 0:257], lhsT=qT[:, csl], rhs=Cb[:, c_, :], start=False, stop=True),
                   reads=[r_q, r_cb], writes=[rps[2]])
        if c_ < NCH - 1:
            P.emit("pe", lambda e, c_=c_: e.matmul(ps[3][:, 0:257], lhsT=ktok[:, c_, :], rhs=Vw[0], start=True, stop=True),
                   reads=[r_kt, r_vw[0]], writes=[rps[3]])
            if c_ == 0:
                P.emit("dve", lambda e: e.tensor_copy(U[1], ps[3][:, 0:257]), reads=[rps[3]], writes=[r_U[1]])
            else:
                P.emit("dve", lambda e, c_=c_: e.scalar_tensor_tensor(out=U[1], in0=U[1], scalar=decF(c_ - 1), in1=ps[3][:, 0:257], op0=ALU.mult, op1=ALU.add),
                       reads=[r_U[1], r_w6, rps[3]], writes=[r_U[1]])
        P.emit("act", lambda e, c_=c_: e.activation(out=dn[:, 0:1], in_=ps[1][:, 256:257], func=AF.Abs, scale=EE[:, c_, 0:1]), reads=[rps[1], r_ee], writes=[r_dn])
        P.emit("act", lambda e, c_=c_: e.activation(out=dn[:, 1:2], in_=ps[2][:, 256:257], func=AF.Abs, scale=EE[:, c_, 1:2]), reads=[rps[2], r_ee], writes=[r_dn])
        P.emit("dve", lambda e: e.tensor_scalar_max(dn[:, 0:2], dn[:, 0:2], 1.0), reads=[r_dn], writes=[r_dn])
        P.emit("dve", lambda e: e.reciprocal(dn[:, 2:4], dn[:, 0:2]), reads=[r_dn], writes=[r_dn])
        P.emit("dve", lambda e, c_=c_: e.tensor_tensor(out=dn[:, 4:6], in0=dn[:, 2:4], in1=EE[:, c_, :], op=ALU.mult), reads=[r_dn, r_ee], writes=[r_dn])
        P.emit("act", lambda e: e.activation(out=hh, in_=ps[1][:, 0:256], func=AF.Copy, scale=dn[:, 4:5]), reads=[rps[1], r_dn], writes=[r_h])
        P.emit("dve", lambda e: e.scalar_tensor_tensor(out=hh, in0=ps[2][:, 0:256], scalar=dn[:, 5:6], in1=hh, op0=ALU.mult, op1=ALU.add),
               reads=[rps[2], r_dn, r_h], writes=[r_h])
        if c_ < NCH - 1:
            P.emit("act", lambda e, c_=c_: e.activation(out=Cf, in_=U[1], func=AF.Copy, scale=decF(c_)), reads=[r_U[1], r_w6], writes=[r_cf])
        P.emit("dve", lambda e: e.memset(dn[:, 6:7], 0.0), writes=[r_dn])
        P.emit("act", lambda e: e.activation(out=junk, in_=hh, func=AF.Square, accum_out=dn[:, 6:7]), reads=[r_h, r_dn], writes=[r_junk, r_dn])
        P.emit("act", lambda e: e.activation(out=dn[:, 7:8], in_=dn[:, 6:7], func=AF.Ln, scale=1.0 / 256.0, bias=G.epsc), reads=[r_dn, G.r_cst], writes=[r_dn])
        P.emit("act", lambda e: e.activation(out=dn[:, 7:8], in_=dn[:, 7:8], func=AF.Exp, scale=-0.5), reads=[r_dn], writes=[r_dn])
        P.emit("dve", lambda e: e.tensor_scalar(hbf, hh, dn[:, 7:8], None, op0=ALU.mult), reads=[r_h, r_dn], writes=[r_hbf])
        sb, cl = (c_ // 4) % 2, c_ % 4
        for et in range(2):
            pi = 4 + et
            P.emit("pe", lambda e, et=et, pi=pi: e.transpose(out=ps_bf[pi][:, 0:128], in_=hbf[:, et * 128:(et + 1) * 128], identity=G.ident),
                   reads=[r_hbf, G.r_cst], writes=[rps[pi]])
            P.emit("dve", lambda e, et=et, pi=pi, sb=sb, cl=cl, csl=csl: e.scalar_tensor_tensor(
                out=stage[sb][:, et, cl * 128:(cl + 1) * 128], in0=ps_bf[pi][:, 0:128], scalar=nws[:, et:et + 1], in1=sigo[:, et, csl],
                op0=ALU.mult, op1=ALU.mult), reads=[rps[pi], r_nw, r_so], writes=[r_st[sb]])
        if cl == 3:
            g_ = c_ // 4
            j_, tl0 = g_ // 4, (g_ % 4) * 512
            dst = outT[j_ * 256:(j_ + 1) * 256, tl0:tl0 + 512].rearrange("(a p) t -> p a t", p=128)
            rec = P.dma("sp", dst, stage[sb], reads=[r_st[sb]])
            if final:
                P.final_waits.append(rec)


def run_mlstm(hn_sh, w_in, b_gate, norm_w):
    nc = _prog(("mlstm",), build_mlstm)
    cst = make_consts()
    maps = []
    for r in range(8):
        b, h = r // 4, r % 4
        hn_full = np.concatenate([hn_sh[4 * b + j] for j in range(4)], axis=0)
        wfm = np.concatenate([w_in[:, h * 128:(h + 1) * 128], w_in[:, 512 + h * 128:512 + (h + 1) * 128],
                              w_in[:, 2048 + h * 256:2048 + (h + 1) * 256]], axis=1)
        gcols = [3072 + h, 3076 + h, 3080 + h, 3084 + h]
        wtk = np.concatenate([w_in[:, 512 + h * 128:512 + (h + 1) * 128], w_in[:, 1024 + h * 256:1024 + (h + 1) * 256],
                              w_in[:, gcols]], axis=1)
        bgv = np.ascontiguousarray(b_gate[[h, 4 + h, 8 + h, 12 + h]].reshape(1, 4))
        nwv = np.ascontiguousarray(norm_w[h * 256:(h + 1) * 256].reshape(2, 128).T)
        maps.append({"cst": cst, "hnT": np.ascontiguousarray(hn_full), "wfm": np.ascontiguousarray(wfm),
                     "wtk": np.ascontiguousarray(wtk), "bg": bgv, "nw": nwv})
    res = run_bass_kernel_spmd(nc, maps, core_ids=list(range(8)))
    outs = [r_["outT"] for r_ in res.results]
    mT = []
    for rr in range(8):
        b, j = rr // 4, rr % 4
        mT.append(np.concatenate([outs[4 * b + h][j * 256:(j + 1) * 256] for h in range(4)], axis=0))
    return mT


def build_attn():
    nc = bass.Bass("TRN2", target_bir_lowering=False)
    cst = nc.dram_tensor("cst", [128, CST_W], F32, kind="ExternalInput").ap()
    hnT = nc.dram_tensor("hnT", [4 * D, T], BF16, kind="ExternalInput").ap()
    wfm = nc.dram_tensor("wfm", [D, 768], F32, kind="ExternalInput").ap()
    wv = nc.dram_tensor("wv", [D, 64], F32, kind="ExternalInput").ap()
    sink = nc.dram_tensor("sink", [1, 4], F32, kind="ExternalInput").ap()
    pos = nc.dram_tensor("pos", [1, S], I32, kind="ExternalInput").ap()
    outT = nc.dram_tensor("outT", [4 * 256, T], BF16, kind="ExternalOutput").ap()
    G = Ctx(nc, cst)
    emit_attn(G, hnT, wfm, wv, sink, pos, outT, final=True)
    G.P.build()
    return nc


def emit_attn(G, hnT, wfm, wv, sink, pos, outT, final=False):
    P, A = G.P, G.A
    NB = 64
    cosT = A.alloc([128, S], F32)
    sinS = A.alloc([128, S], F32)
    qT = A.alloc([128, 2, S], BF16)
    kT2 = A.alloc([128, S], BF16)
    vext = A.alloc([128, NB, 65], BF16)
    hnb = [A.alloc([128, 8, 512], BF16) for _ in range(2)]
    wfm_s = A.alloc([128, 8, 768], BF16)
    wv_s = A.alloc([128, 8, 64], BF16)
    PIECE = 1024
    posi = A.alloc([128, PIECE], I32)
    ang = A.alloc([128, PIECE], F32)
    ki = A.alloc([128, PIECE], I32)
    kf = A.alloc([128, PIECE], F32)
    rr_ = A.alloc([128, PIECE], F32)
    t1 = [A.alloc([128, 512], F32) for _ in range(2)]
    t2 = [A.alloc([128, 512], F32) for _ in range(2)]
    ppe = A.alloc([128, 4, 384], BF16)
    pm = [A.alloc([128, 4, 384], BF16) for _ in range(3)]
    esk = A.alloc([128, 4], F32)
    dn4 = A.alloc([128, 8], F32)
    otok = A.alloc([128, 256], BF16)
    stage = [A.alloc([128, 2, 512], BF16) for _ in range(2)]
    R = Res
    r_cos, r_sin, r_q, r_k, r_v, r_w = [R(n) for n in "cos sin q k v w".split()]
    r_hnb = [R("hnb0"), R("hnb1")]
    r_posi, r_ang, r_ki, r_kf, r_rr = [R(n) for n in "posi ang ki kf rr".split()]
    r_t1 = [R("t10"), R("t11")]
    r_t2 = [R("t20"), R("t21")]
    r_ppe = R("ppe")
    r_pm = [R("pm0"), R("pm1"), R("pm2")]
    r_esk, r_dn4, r_otok = R("esk"), R("dn4"), R("otok")
    r_st = [R("st0"), R("st1")]
    ps, rps = G.ps, G.rps
    ps_bf = [p_[:, :].bitcast(BF16) for p_ in ps]

    wf_v = wfm.rearrange("(c p) f -> p c f", p=128)
    P.dma("pool", wfm_s[:, 0:4], wf_v[:, 0:4], writes=[r_w])
    P.dma("pool", wfm_s[:, 4:8], wf_v[:, 4:8], writes=[r_w], chain=True)
    P.dma("pool", wv_s, wv.rearrange("(c p) f -> p c f", p=128), writes=[r_w], chain=True)
    P.dma("sp", esk, sink[0:1, :].partition_broadcast(128), writes=[r_esk])
    P.emit("act", lambda e: e.activation(out=esk, in_=esk, func=AF.Exp), reads=[r_esk], writes=[r_esk])
    P.emit("pool", lambda e: e.memset(vext[:, :, 64:65], 1.0), writes=[r_v])

    def reduce_sin(dst, src_ang, shift, sc, r_dst, psl):
        if shift != 0.0:
            P.emit("dve", lambda e: e.tensor_scalar(rr_, src_ang, shift, None, op0=ALU.add), reads=[r_ang], writes=[r_rr])
            base, r_base = rr_, r_rr
        else:
            base, r_base = src_ang, r_ang
        P.emit("dve", lambda e: e.tensor_scalar(ki, base, 1.0 / TWO_PI, None, op0=ALU.mult), reads=[r_base], writes=[r_ki])
        P.emit("dve", lambda e: e.tensor_copy(kf, ki), reads=[r_ki], writes=[r_kf])
        P.emit("dve", lambda e: e.scalar_tensor_tensor(out=rr_, in0=kf, scalar=-C1, in1=base, op0=ALU.mult, op1=ALU.add),
               reads=[r_kf, r_base], writes=[r_rr])
        P.emit("dve", lambda e: e.scalar_tensor_tensor(out=rr_, in0=kf, scalar=-C2, in1=rr_, op0=ALU.mult, op1=ALU.add),
               reads=[r_kf, r_rr], writes=[r_rr])
        P.emit("dve", lambda e: e.tensor_scalar(rr_, rr_, PI_SAFE, -PI_SAFE, op0=ALU.min, op1=ALU.max), reads=[r_rr], writes=[r_rr])
        if sc is None:
            P.emit("act", lambda e: e.activation(out=dst[:, psl], in_=rr_, func=AF.Sin), reads=[r_rr], writes=[r_dst])
        else:
            P.emit("act", lambda e: e.activation(out=dst[:, psl], in_=rr_, func=AF.Sin, scale=sc), reads=[r_rr, G.r_cst], writes=[r_dst])

    for pc in range(S // PIECE):
        psl = slice(pc * PIECE, (pc + 1) * PIECE)
        P.dma("sp", posi, pos[0:1, psl].partition_broadcast(128), writes=[r_posi])
        P.emit("dve", lambda e: e.tensor_copy(ang, posi), reads=[r_posi], writes=[r_ang])
        P.emit("dve", lambda e: e.tensor_scalar(ang, ang, G.invf, None, op0=ALU.mult), reads=[r_ang, G.r_cst], writes=[r_ang])
        reduce_sin(sinS, ang, 0.0, G.sgn, r_sin, psl)
        reduce_sin(cosT, ang, PI / 2.0, None, r_cos, psl)

    kk = 0
    for tg in range(16):
        b = tg % 2
        rr, tl0 = tg // 4, (tg % 4) * 512
        src = hnT[rr * D:(rr + 1) * D, tl0:tl0 + 512].rearrange("(c p) t -> p c t", p=128)
        P.dma("sp", hnb[b], src, writes=[r_hnb[b]])
        tsl = slice(tg * 512, (tg + 1) * 512)
        for combo, (fa, fb) in enumerate(((0, 2), (1, 3), (4, 5))):
            pa, pb = (kk % 2) * 2, (kk % 2) * 2 + 1
            ti = kk % 2
            kk += 1
            for ft, pi in ((fa, pa), (fb, pb)):
                for ch in range(8):
                    P.emit("pe", lambda e, ch=ch, ft=ft, pi=pi, b=b: e.matmul(
                        ps[pi][:, :], lhsT=wfm_s[:, ch, ft * 128:(ft + 1) * 128], rhs=hnb[b][:, ch, :], start=(ch == 0), stop=(ch == 7)),
                        reads=[r_w, r_hnb[b]], writes=[rps[pi]], inc=(ch == 7))
            P.emit("dve", lambda e, pa=pa, ti=ti, tsl=tsl: e.tensor_tensor(out=t1[ti], in0=ps[pa][:, :], in1=cosT[:, tsl], op=ALU.mult),
                   reads=[rps[pa], r_cos], writes=[r_t1[ti]])
            P.emit("dve", lambda e, pb=pb, ti=ti, tsl=tsl: e.tensor_tensor(out=t2[ti], in0=ps[pb][:, :], in1=sinS[:, tsl], op=ALU.mult),
                   reads=[rps[pb], r_sin], writes=[r_t2[ti]])
            if combo < 2:
                P.emit("pool", lambda e, ti=ti, combo=combo, tsl=tsl: e.tensor_tensor(out=qT[:, combo, tsl], in0=t1[ti], in1=t2[ti], op=ALU.add),
                       reads=[r_t1[ti], r_t2[ti]], writes=[r_q])
            else:
                P.emit("pool", lambda e, ti=ti, tsl=tsl: e.tensor_tensor(out=kT2[:, tsl], in0=t1[ti], in1=t2[ti], op=ALU.add),
                       reads=[r_t1[ti], r_t2[ti]], writes=[r_k])
        for cl in range(4):
            c_ = tg * 4 + cl
            pi = 4 + (c_ % 2)
            for ch in range(8):
                P.emit("pe", lambda e, ch=ch, cl=cl, pi=pi, b=b: e.matmul(
                    ps[pi][:, 0:64], lhsT=hnb[b][:, ch, cl * 128:(cl + 1) * 128], rhs=wv_s[:, ch, :], start=(ch == 0), stop=(ch == 7)),
                    reads=[r_w, r_hnb[b]], writes=[rps[pi]], inc=(ch == 7))
            P.emit("act", lambda e, pi=pi, c_=c_: e.activation(out=vext[:, c_, 0:64], in_=ps[pi][:, 0:64], func=AF.Copy),
                   reads=[rps[pi]], writes=[r_v])

    def finalize(j):
        po = 4 + (j % 2)
        kbs = [kb for kb in (j - 1, j, j + 1) if 0 <= kb < NB]
        for h in range(4):
            for i_, kb in enumerate(kbs):
                qoff = (j - max(kb - 1, 0)) * 128
                P.emit("pe", lambda e, h=h, kb=kb, qoff=qoff, po=po, i_=i_: e.matmul(
                    ps[po][:, h * 65:(h + 1) * 65], lhsT=pm[kb % 3][:, h, qoff:qoff + 128], rhs=vext[:, kb, :],
                    start=(i_ == 0), stop=(i_ == len(kbs) - 1)),
                    reads=[r_pm[kb % 3], r_v], writes=[rps[po]], inc=(i_ == len(kbs) - 1))
        pv = ps[po][:, 0:260].rearrange("p (h e) -> p h e", e=65)
        P.emit("dve", lambda e: e.tensor_tensor(out=dn4[:, 0:4], in0=pv[:, :, 64], in1=esk, op=ALU.add), reads=[rps[po], r_esk], writes=[r_dn4])
        P.emit("dve", lambda e: e.reciprocal(dn4[:, 4:8], dn4[:, 0:4]), reads=[r_dn4], writes=[r_dn4])
        for h in range(4):
            P.emit("act", lambda e, h=h: e.activation(out=otok[:, h * 64:(h + 1) * 64], in_=pv[:, h, 0:64], func=AF.Copy, scale=dn4[:, 4 + h:5 + h]),
                   reads=[rps[po], r_dn4], writes=[r_otok])
        sb, cl = (j // 4) % 2, j % 4
        for et in range(2):
            pi = 6 + et
            P.emit("pe", lambda e, et=et, pi=pi: e.transpose(out=ps_bf[pi][:, 0:128], in_=otok[:, et * 128:(et + 1) * 128], identity=G.ident),
                   reads=[r_otok, G.r_cst], writes=[rps[pi]])
            P.emit("dve", lambda e, et=et, pi=pi, sb=sb, cl=cl: e.tensor_copy(stage[sb][:, et, cl * 128:(cl + 1) * 128], ps_bf[pi][:, 0:128]),
                   reads=[rps[pi]], writes=[r_st[sb]])
        if cl == 3:
            g_ = j // 4
            j_, tl0 = g_ // 4, (g_ % 4) * 512
            dst = outT[j_ * 256:(j_ + 1) * 256, tl0:tl0 + 512].rearrange("(a p) t -> p a t", p=128)
            rec = P.dma("sp", dst, stage[sb], reads=[r_st[sb]])
            if final:
                P.final_waits.append(rec)

    for kb in range(NB):
        qb0 = max(kb - 1, 0)
        qb1 = min(kb + 1, NB - 1)
        nq = (qb1 - qb0 + 1) * 128
        r0 = 0 if kb > 0 else 128
        ksl = slice(kb * 128, (kb + 1) * 128)
        qsl = slice(qb0 * 128, qb0 * 128 + nq)
        for h in range(4):
            pr, hh_ = h // 2, h % 2
            psl_ = slice(hh_ * 64, (hh_ + 1) * 64)
            P.emit("pe", lambda e, h=h, pr=pr, psl_=psl_, ksl=ksl, qsl=qsl, nq=nq: e.matmul(
                ps[h][:, 0:nq], lhsT=kT2[psl_, ksl], rhs=qT[psl_, pr, qsl], start=True, stop=True),
                reads=[r_k, r_q], writes=[rps[h]])
            P.emit("act", lambda e, h=h, nq=nq: e.activation(out=ppe[:, h, 0:nq], in_=ps[h][:, 0:nq], func=AF.Exp, scale=0.125),
                   reads=[rps[h]], writes=[r_ppe])
            P.emit("pool", lambda e, h=h, nq=nq, r0=r0, kb=kb: e.tensor_tensor(out=pm[kb % 3][:, h, 0:nq], in0=ppe[:, h, 0:nq],
                                                                        in1=G.band_bf[:, r0:r0 + nq], op=ALU.mult),
                   reads=[r_ppe, G.r_cst], writes=[r_pm[kb % 3]])
        if kb >= 1:
            finalize(kb - 1)
    finalize(NB - 1)


def run_attn(hn_sh, w_in, sink, positions):
    nc = _prog(("attn",), build_attn)
    cst = make_consts()
    maps = []
    for r in range(8):
        b, kv = r // 4, r % 4
        hn_full = np.concatenate([hn_sh[4 * b + j] for j in range(4)], axis=0)
        q = w_in[:, kv * 256:(kv + 1) * 256]
        qp = q.reshape(D, 4, 2, 32)[:, :, ::-1, :].reshape(D, 256)
        k = w_in[:, 1024 + kv * 64:1024 + (kv + 1) * 64]
        kp = k.reshape(D, 2, 32)[:, ::-1, :].reshape(D, 64)
        wfm = np.concatenate([q, qp, k, k, kp, kp], axis=1)
        wvv = w_in[:, 1280 + kv * 64:1280 + (kv + 1) * 64]
        maps.append({"cst": cst, "hnT": np.ascontiguousarray(hn_full), "wfm": np.ascontiguousarray(wfm),
                     "wv": np.ascontiguousarray(wvv), "sink": np.ascontiguousarray(sink[kv * 4:(kv + 1) * 4].reshape(1, 4)),
                     "pos": np.ascontiguousarray(positions[b].reshape(1, S).astype(np.int32))})
    res = run_bass_kernel_spmd(nc, maps, core_ids=list(range(8)))
    outs = [r_["outT"] for r_ in res.results]
    mT = []
    for rr in range(8):
        b, j = rr // 4, rr % 4
        mT.append(np.concatenate([outs[4 * b + h][j * 256:(j + 1) * 256] for h in range(4)], axis=0))
    return mT


def kernel(x, positions, norm_mix_w, norm_ffn_w, norm_final_w, mlstm_w_in, mlstm_b_gate, mlstm_norm_w,
           mlstm_w_out, attn_w_in, attn_sink, attn_w_out, ffn_w_in, ffn_w_out):
    f = lambda a: np.asarray(a, np.float32)
    x = f(x).reshape(16384, D)
    xT = [np.ascontiguousarray(x[r * T:(r + 1) * T].T) for r in range(8)]
    positions = np.asarray(positions)
    xT, hn = run_tp(xT, None, None, None, None, None, f(norm_mix_w)[0], False)
    for i in range(4):
        j = i // 2
        if i % 2 == 0:
            mT = run_mlstm(hn, f(mlstm_w_in)[j], f(mlstm_b_gate)[j], f(mlstm_norm_w)[j])
            wo = f(mlstm_w_out)[j]
        else:
            mT = run_attn(hn, f(attn_w_in)[j], f(attn_sink)[j], positions)
            wo = f(attn_w_out)[j]
        if i < 3:
            xT, hn = run_tp(xT, mT, wo, f(ffn_w_in)[i], f(ffn_w_out)[i], f(norm_ffn_w)[i], f(norm_mix_w)[i + 1], False)
        else:
            yT = run_tp(xT, mT, wo, f(ffn_w_in)[i], f(ffn_w_out)[i], f(norm_ffn_w)[i], f(norm_final_w), True)
    y = np.concatenate([np.asarray(t).T for t in yT], axis=0).reshape(2, S, D).astype(np.float32)
    return y
```
